# Optimizing a Trainium2 kernel written in Bass

```python
import jax, jax.numpy as jnp
from jax import lax
import numpy as np

D_MODEL = 4096
BATCH = 4
SEQ = 4096
DEPTH = 1

CHUNK = 64
N_MEM = 256
D_MIX = D_MODEL
D_GMLP = D_MIX // 2
D_HGRN = D_MIX - D_GMLP
GMLP_HEADS = 4
GMLP_HEAD_DIM = D_GMLP // GMLP_HEADS
GMLP_BLOCK = 128
HGRN_HEAD_DIM = 128
HGRN_HEADS = D_HGRN // HGRN_HEAD_DIM
X_HEADS = 4
X_HEAD_DIM = D_MODEL // X_HEADS
N_GROUPS = 4
EXPERTS_PER_GROUP = 8
N_EXPERTS = N_GROUPS * EXPERTS_PER_GROUP
TOP_K = 2
D_EXPERT = D_MODEL // 4
MOE_BLOCK = 128
EPS = 1e-6
IN_WIDTHS = (D_GMLP, D_GMLP, D_HGRN, D_HGRN, D_HGRN, D_HGRN)
D_IN = sum(IN_WIDTHS)

kernel_name = "hybrid_gmlp_hgrn2_xattn_hmoe_block"


def rms_norm(x, g):
    xf = x.astype(jnp.float32)
    y = xf * lax.rsqrt(jnp.mean(xf * xf, axis=-1, keepdims=True) + EPS)
    return (y * g.astype(jnp.float32)).astype(x.dtype)


def layer_norm(x, g, b):
    xf = x.astype(jnp.float32)
    mu = jnp.mean(xf, axis=-1, keepdims=True)
    xc = xf - mu
    var = jnp.mean(xc * xc, axis=-1, keepdims=True)
    y = xc * lax.rsqrt(var + EPS) * g.astype(jnp.float32) + b.astype(jnp.float32)
    return y.astype(x.dtype)


def gmlp_mixer(u, v, ln_g, ln_b, w_s, b_s):
    B, S, _ = u.shape
    nb = S // GMLP_BLOCK
    v = v.reshape(B, S, GMLP_HEADS, GMLP_HEAD_DIM)
    v = layer_norm(v, ln_g.reshape(GMLP_HEADS, GMLP_HEAD_DIM), ln_b.reshape(GMLP_HEADS, GMLP_HEAD_DIM))
    v = v.reshape(B, nb, GMLP_BLOCK, GMLP_HEADS, GMLP_HEAD_DIM)
    cpos = jnp.arange(GMLP_BLOCK) // CHUNK
    mask = cpos[None, :] <= cpos[:, None]
    w = jnp.where(mask[None], w_s, 0.0)
    s = jnp.einsum('hij,bnjhc->bnihc', w, v) + b_s.T[None, None, :, :, None]
    return u * s.reshape(B, S, D_GMLP)


def hgrn2_mixer(q, f, i, g, lb, norm_g):
    B, S, _ = q.shape
    H, K, C = HGRN_HEADS, HGRN_HEAD_DIM, CHUNK
    nc = S // C
    f32 = jnp.float32

    def heads(t):
        return t.reshape(B, nc, C, H, K).transpose(0, 3, 1, 2, 4)

    fg = lb + (1.0 - lb) * jax.nn.sigmoid(f.astype(f32))
    qh = heads(jax.nn.silu(q.astype(f32)))
    kh = heads(1.0 - fg)
    vh = heads(i.astype(f32))
    b = jnp.cumsum(heads(jnp.log(fg)), axis=3)
    b_last = b[:, :, :, -1:, :]
    b_mid = b[:, :, :, C // 2:C // 2 + 1, :]

    a = jnp.einsum('bhntk,bhnsk->bhnts', qh * jnp.exp(b - b_mid), kh * jnp.exp(b_mid - b))
    causal = jnp.tril(jnp.ones((C, C), dtype=bool))
    a = jnp.where(causal, a, 0.0)
    o_intra = jnp.einsum('bhnts,bhnsv->bhntv', a, vh)

    incr = jnp.einsum('bhnsk,bhnsv->bhnkv', kh * jnp.exp(b_last - b), vh)
    decay = jnp.exp(b_last[:, :, :, 0, :])

    def step(state, xs):
        inc_n, d_n = xs
        return d_n[..., None] * state + inc_n, state

    s0 = jnp.zeros((B, H, K, K), f32)
    _, s_start = lax.scan(step, s0, (jnp.moveaxis(incr, 2, 0), jnp.moveaxis(decay, 2, 0)))
    s_start = jnp.moveaxis(s_start, 0, 2)
    o_inter = jnp.einsum('bhntk,bhnkv->bhntv', qh * jnp.exp(b), s_start)

    o = (o_intra + o_inter).transpose(0, 2, 3, 1, 4).reshape(B, S, H, K)
    o = rms_norm(o, norm_g.reshape(H, K)).reshape(B, S, D_HGRN)
    o = o * jax.nn.silu(g.astype(f32))
    return o.astype(q.dtype)


def cross_attention(hn, mem_n, w_q, w_k, w_v, w_o):
    B, S, _ = hn.shape
    M = mem_n.shape[1]
    q = (hn @ w_q).reshape(B, S, X_HEADS, X_HEAD_DIM)
    k = (mem_n @ w_k).reshape(B, M, X_HEADS, X_HEAD_DIM)
    v = (mem_n @ w_v).reshape(B, M, X_HEADS, X_HEAD_DIM)
    s = jnp.einsum('bqhd,bmhd->bhqm', q, k).astype(jnp.float32) * (X_HEAD_DIM ** -0.5)
    p = jax.nn.softmax(s, axis=-1).astype(v.dtype)
    o = jnp.einsum('bhqm,bmhd->bqhd', p, v).reshape(B, S, D_MODEL)
    return o @ w_o


def hier_moe(xn, w_group, b_group, w_router, b_router, w1, w3, w2):
    B, S, D = xn.shape
    T = B * S
    f32 = jnp.float32
    xt = xn.reshape(T, D)
    g_logits = (xt @ w_group).astype(f32) + b_group.astype(f32)
    g_prob = jax.nn.softmax(g_logits, axis=-1)
    grp = jnp.argmax(g_logits, axis=-1).astype(jnp.int32)
    p_grp = jnp.take_along_axis(g_prob, grp[:, None], axis=-1)[:, 0]
    e_all = jnp.einsum('td,gde->tge', xt, w_router).astype(f32) + b_router.astype(f32)
    e_logits = jnp.take_along_axis(e_all, grp[:, None, None], axis=1)[:, 0]
    top_v, top_i = lax.top_k(e_logits, TOP_K)
    gate = p_grp[:, None] * jax.nn.softmax(top_v, axis=-1)
    eid = grp[:, None] * EXPERTS_PER_GROUP + top_i.astype(jnp.int32)

    A = T * TOP_K
    eid_f = eid.reshape(A)
    tok_f = jnp.repeat(jnp.arange(T, dtype=jnp.int32), TOP_K)
    gate_f = gate.reshape(A)
    order = jnp.argsort(eid_f, stable=True)
    se = eid_f[order]
    counts = jnp.bincount(eid_f, length=N_EXPERTS)
    start = jnp.cumsum(counts) - counts
    pcounts = (counts + MOE_BLOCK - 1) // MOE_BLOCK * MOE_BLOCK
    pend = jnp.cumsum(pcounts)
    pstart = pend - pcounts
    dest = pstart[se] + jnp.arange(A, dtype=jnp.int32) - start[se]
    n_blocks = -(-(A + N_EXPERTS * (MOE_BLOCK - 1)) // MOE_BLOCK)
    P = n_blocks * MOE_BLOCK
    slot_tok = jnp.full((P,), T, jnp.int32).at[dest].set(tok_f[order])
    slot_gate = jnp.zeros((P,), f32).at[dest].set(gate_f[order])
    block_eid = jnp.minimum(
        jnp.searchsorted(pend, jnp.arange(n_blocks) * MOE_BLOCK, side='right'), N_EXPERTS - 1)

    x_pad = jnp.concatenate([xt, jnp.zeros((1, D), xt.dtype)], axis=0)
    xb = x_pad[slot_tok].reshape(n_blocks, MOE_BLOCK, D)

    def expert_block(args):
        e, xblk = args
        hdn = jax.nn.silu(xblk @ w1[e]) * (xblk @ w3[e])
        return hdn @ w2[e]

    yb = lax.map(expert_block, (block_eid, xb)).reshape(P, D)
    out = jnp.zeros((T + 1, D), xn.dtype).at[slot_tok].add(yb * slot_gate[:, None].astype(yb.dtype))
    return out[:T].reshape(B, S, D)


def setup_inputs(seed: int = 0) -> dict:
    key = jax.random.key(seed)
    ks = jax.random.split(key, 32)
    L = DEPTH
    nrm = jax.random.normal
    f32 = jnp.float32

    def gain(k, shape):
        return 1.0 + 0.02 * nrm(k, shape, f32)

    return {
        "x": nrm(ks[0], (BATCH, SEQ, D_MODEL), f32),
        "mem": nrm(ks[1], (BATCH, N_MEM, D_MODEL), f32),
        "n_mix": gain(ks[2], (L, D_MODEL)),
        "w_in": nrm(ks[3], (L, D_MODEL, D_IN), f32) * D_MODEL ** -0.5,
        "gmlp_ln_g": gain(ks[4], (L, D_GMLP)),
        "gmlp_ln_b": 0.02 * nrm(ks[5], (L, D_GMLP), f32),
        "gmlp_w_s": nrm(ks[6], (L, GMLP_HEADS, GMLP_BLOCK, GMLP_BLOCK), f32) * GMLP_BLOCK ** -0.5,
        "gmlp_b_s": 1.0 + 0.1 * nrm(ks[7], (L, GMLP_HEADS, GMLP_BLOCK), f32),
        "hgrn_lower_bounds": 0.1 * nrm(ks[8], (L + 1, D_HGRN), f32),
        "hgrn_norm_g": gain(ks[9], (L, D_HGRN)),
        "w_out": nrm(ks[10], (L, D_MIX, D_MODEL), f32) * D_MIX ** -0.5,
        "n_cross": gain(ks[11], (L, D_MODEL)),
        "n_mem": gain(ks[12], (L, D_MODEL)),
        "w_q_x": nrm(ks[13], (L, D_MODEL, D_MODEL), f32) * D_MODEL ** -0.5,
        "w_k_x": nrm(ks[14], (L, D_MODEL, D_MODEL), f32) * D_MODEL ** -0.5,
        "w_v_x": nrm(ks[15], (L, D_MODEL, D_MODEL), f32) * D_MODEL ** -0.5,
        "w_o_x": nrm(ks[16], (L, D_MODEL, D_MODEL), f32) * D_MODEL ** -0.5,
        "n_moe": gain(ks[17], (L, D_MODEL)),
        "w_group": nrm(ks[18], (L, D_MODEL, N_GROUPS), f32) * D_MODEL ** -0.5,
        "b_group": 0.01 * nrm(ks[19], (L, N_GROUPS), f32),
        "w_router": nrm(ks[20], (L, N_GROUPS, D_MODEL, EXPERTS_PER_GROUP), f32) * D_MODEL ** -0.5,
        "b_router": 0.01 * nrm(ks[21], (L, N_GROUPS, EXPERTS_PER_GROUP), f32),
        "w1_e": nrm(ks[22], (L, N_EXPERTS, D_MODEL, D_EXPERT), f32) * D_MODEL ** -0.5,
        "w3_e": nrm(ks[23], (L, N_EXPERTS, D_MODEL, D_EXPERT), f32) * D_MODEL ** -0.5,
        "w2_e": nrm(ks[24], (L, N_EXPERTS, D_EXPERT, D_MODEL), f32) * D_EXPERT ** -0.5,
        "n_final": gain(ks[25], (D_MODEL,)),
    }


def reference(x, mem, n_mix, w_in, gmlp_ln_g, gmlp_ln_b, gmlp_w_s, gmlp_b_s, hgrn_lower_bounds,
              hgrn_norm_g, w_out, n_cross, n_mem, w_q_x, w_k_x, w_v_x, w_o_x, n_moe, w_group,
              b_group, w_router, b_router, w1_e, w3_e, w2_e, n_final):
    split_at = [int(c) for c in np.cumsum(IN_WIDTHS)[:-1]]
    lb_all = jnp.cumsum(jax.nn.softmax(hgrn_lower_bounds.astype(jnp.float32), axis=0), axis=0)
    h = x
    for l in range(DEPTH):
        hn = rms_norm(h, n_mix[l])
        z = hn @ w_in[l]
        u, v, q, f, i, g = jnp.split(z, split_at, axis=-1)
        y_a = gmlp_mixer(jax.nn.gelu(u, approximate=False), jax.nn.gelu(v, approximate=False),
                         gmlp_ln_g[l], gmlp_ln_b[l], gmlp_w_s[l], gmlp_b_s[l])
        y_b = hgrn2_mixer(q, f, i, g, lb_all[l], hgrn_norm_g[l])
        h = h + jnp.concatenate([y_a, y_b], axis=-1) @ w_out[l]
        h = h + cross_attention(rms_norm(h, n_cross[l]), rms_norm(mem, n_mem[l]),
                                w_q_x[l], w_k_x[l], w_v_x[l], w_o_x[l])
        h = h + hier_moe(rms_norm(h, n_moe[l]), w_group[l], b_group[l], w_router[l], b_router[l],
                         w1_e[l], w3_e[l], w2_e[l])
    return rms_norm(h, n_final)
```

```python
import numpy as np
import concourse.bass as bass
import concourse.mybir as mybir
from concourse.bass_utils import run_bass_kernel_spmd

F32, BF16, I32 = mybir.dt.float32, mybir.dt.bfloat16, mybir.dt.int32
AF = mybir.ActivationFunctionType
ALU = mybir.AluOpType
AX = mybir.AxisListType
NCORES = 8
import os
KDUP = int(os.environ.get('KDUP', '1'))
KFENCE = int(os.environ.get('KFENCE', '1'))
KREG = int(os.environ.get('KREG', '1'))
KSTOP = int(os.environ.get('KSTOP', '0'))
KSKIP = os.environ.get('KSKIP', '')
KNCC = int(os.environ.get('KNCC', '99'))
GATHER = int(os.environ.get('KGATHER', '1'))
EPS = 1e-6
ENGS = ["pe", "act", "dve", "pool", "sp"]


def make_cfg(D=4096, SEQ=4096, BATCH=4, CAP=256):
    c = dict(D=D, SEQ=SEQ, BATCH=BATCH, CAP=CAP)
    c["KC"] = D // 128
    c["TOK"] = BATCH * SEQ // NCORES
    c["NT"] = c["TOK"] // 128
    c["SBT"] = 4
    c["NSB"] = c["NT"] // 4
    c["DH"] = D // 2
    c["GH"] = c["DH"] // 512
    c["HH"] = c["DH"] // 128
    c["XH"] = 4
    c["XKC"] = D // 4 // 128
    c["NE"] = 32
    c["FE"] = D // 4
    c["FKC"] = c["FE"] // 128
    c["NMEM"] = 256
    return c


class Sched:
    NS = 16
    EP = 1000
    NCSEM = 12

    def __init__(self, nc):
        self.nc = nc
        self.ops = []
        self.last_w = {}
        self.readers = {}
        self.cnt = {e: 0 for e in ENGS}
        self.csem = {e: [nc.alloc_semaphore(f"c_{e}_{k}") for k in range(self.NCSEM)] for e in ENGS if e != "sp"}
        self.dq = {e: dict(rr=0, sems=[nc.alloc_semaphore(f"d_{e}_{k}") for k in range(self.NS)], tot=[0] * self.NS)
                   for e in ("sp", "pool")}
        self.ccsem = nc.alloc_semaphore("ccsem")
        self.cctot = 0

    def add(self, eng, fn, r=(), w=(), kind="c"):
        i = len(self.ops)
        deps = set()
        for k in r:
            if k in self.last_w:
                deps.add(self.last_w[k])
        for k in w:
            if k in self.last_w:
                deps.add(self.last_w[k])
            deps.update(self.readers.get(k, ()))
        for k in r:
            self.readers.setdefault(k, []).append(i)
        for k in w:
            self.last_w[k] = i
            self.readers[k] = []
        deps.discard(i)
        self.ops.append(dict(eng=eng, fn=fn, deps=deps, kind=kind, sig=False, done=None, pre=None))
        return i

    def flush(self):
        nc = self.nc
        ops = self.ops
        for op in ops:
            for d in op["deps"]:
                ops[d]["sig"] = True
        per = {e: [] for e in ENGS}
        for i, op in enumerate(ops):
            per[op["eng"]].append(i)
        for e in ENGS:
            for i in per[e]:
                op = ops[i]
                if op["kind"] == "d":
                    q = self.dq[e]
                    s = q["rr"] % self.NS
                    q["rr"] += 1
                    op["pre"] = (q["sems"][s], q["tot"][s])
                    q["tot"][s] += 16
                    op["done"] = (q["sems"][s], q["tot"][s], 16)
                elif op["kind"] == "cc":
                    self.cctot += 1
                    op["done"] = (self.ccsem, self.cctot, 1)
                elif op["sig"]:
                    self.cnt[e] += 1
                    k = self.cnt[e] - 1
                    op["done"] = (self.csem[e][k // self.EP], k % self.EP + 1, 1)
        sched = self

        def body_for(e):
            def body(eng):
                waited = {}
                for i in per[e]:
                    op = ops[i]
                    waits = [ops[d]["done"][:2] for d in sorted(op["deps"])]
                    if op["pre"] is not None:
                        waits.append(op["pre"])
                    for sem, val in waits:
                        if val <= 0:
                            continue
                        key = id(sem)
                        if waited.get(key, 0) >= val:
                            continue
                        eng.wait_ge(sem, val)
                        waited[key] = val
                    ins = op["fn"](eng)
                    if op["done"] is not None:
                        ins.then_inc(op["done"][0], op["done"][2])
                if e in sched.dq:
                    q = sched.dq[e]
                    for s in range(sched.NS):
                        if q["tot"][s] > 0:
                            eng.wait_ge(q["sems"][s], q["tot"][s])
                if e == "pool" and sched.cctot > 0:
                    eng.wait_ge(sched.ccsem, sched.cctot)
            return body

        with nc.Block() as block:
            deco = dict(pe=block.tensor, act=block.scalar, dve=block.vector, pool=block.gpsimd, sp=block.sync)
            for e in ENGS:
                if per[e] or e in ("sp", "pool"):
                    deco[e](body_for(e))
        self.ops = []
        self.last_w = {}
        self.readers = {}


def const_table(cfg):
    CAP = cfg["CAP"]
    s = np.arange(128)[:, None]
    t = np.arange(128)[None, :]
    REF = 63
    tabs = {}
    tabs["ident"] = (s == t).astype(np.float32)
    tabs["M1"] = (s <= t).astype(np.float32) - (s <= REF).astype(np.float32)
    mcol = np.zeros((128, 4), np.float32)
    mcol[:, 0] = (np.arange(128) <= REF)
    mcol[:, 1] = 1.0
    mcol[:, 2] = (np.arange(128) > REF)
    tabs["Mcol"] = mcol
    tabs["causT"] = (s <= t).astype(np.float32)
    tabs["gmaskT"] = ((s // 64) <= (t // 64)).astype(np.float32)
    tabs["Lstrict"] = (s < t).astype(np.float32)
    tabs["ones"] = np.ones((128, 128), np.float32)
    tabs["eoff"] = np.broadcast_to((np.arange(32) * CAP).astype(np.float32)[None, :], (128, 32)).copy()
    offs = {}
    o = 0
    for k, v in tabs.items():
        offs[k] = (o, v.shape[1])
        o += v.shape[1]
    return np.concatenate(list(tabs.values()), axis=1), offs


def build(cfg):
    D, KC, TOK, NT, SBT, NSB = cfg["D"], cfg["KC"], cfg["TOK"], cfg["NT"], cfg["SBT"], cfg["NSB"]
    DH, GH, HH, XH, XKC = cfg["DH"], cfg["GH"], cfg["HH"], cfg["XH"], cfg["XKC"]
    NE, FE, FKC, CAP, NMEM = cfg["NE"], cfg["FE"], cfg["FKC"], cfg["CAP"], cfg["NMEM"]
    SBTOK = SBT * 128
    CT = CAP // 128
    ctab, coff = const_table(cfg)
    NCONST = ctab.shape[1]

    nc = bass.Bass("TRN2", target_bir_lowering=False)
    S = Sched(nc)

    def din(name, shape, dt=F32):
        return nc.dram_tensor(name, list(shape), dt, kind="ExternalInput")

    x_own = din("x_own", [TOK, D])
    x_pre = din("x_pre", [TOK, D])
    mem = din("mem", [NMEM, D])
    wsh = {}
    wfull = {}
    wshape = dict(w_in=(D, 3 * D), w_out=(D, D), w_q=(D, D), w_k=(D, D), w_v=(D, D), w_o=(D, D),
                  w1=(NE * D, FE), w3=(NE * D, FE), w2=(NE * FE, D))
    for k, (r_, c_) in wshape.items():
        wsh[k] = din(k + "_s", [r_ // NCORES, c_] if GATHER else [r_, c_])
    gcols = din("gcols", [128, 4 * KC])
    nmoe_b = din("nmoe_b", [128, D])
    nfin_b = din("nfin_b", [128, D])
    lnp = din("lnp", [128, 5 * DH])
    wsT = din("wsT", [128, GH * 128])
    bsc = din("bsc", [128, GH])
    wr_in = din("wr", [D, 36])
    br_in = din("br", [128, 36])
    consts = din("consts", [128, NCONST])
    out = nc.dram_tensor("out", [TOK, D], F32, kind="ExternalOutput")

    wbn = {}
    wfe = {}
    for k, (r_, c_) in wshape.items():
        if not GATHER:
            wfull[k] = wsh[k]
            continue
        if k in ("w1", "w3", "w2"):
            wbn[k] = [nc.dram_tensor(f"{k}_b{g}", [r_ // 32, c_], F32) for g in range(4)]
            wfe[k] = [nc.dram_tensor(f"{k}_f{g}", [r_ // 4, c_], F32) for g in range(4)]
        else:
            wbn[k] = nc.dram_tensor(k + "_b", [r_ // NCORES, c_], F32)
            wfull[k] = nc.dram_tensor(k + "_f", [r_, c_], F32)

    def wexp(k, ex):
        per = wshape[k][0] // NE
        if GATHER:
            return wfe[k][ex // 8], (ex % 8) * per
        return wsh[k], ex * per
    dbg = dict(kind="ExternalOutput") if cfg.get("DEBUG") else {}
    y_d = nc.dram_tensor("y_d", [TOK, D], BF16, **dbg)
    h1_d = nc.dram_tensor("h1_d", [TOK, D], F32, **dbg)
    h2_d = nc.dram_tensor("h2_d", [TOK, D], F32, **dbg)
    xg_d = nc.dram_tensor("xg_d", [NE * CAP, D], BF16)
    yg_d = nc.dram_tensor("yg_d", [NE * CAP, D], F32)
    fence_d = nc.dram_tensor("fence_d", [128, 64], F32)

    def sb(name, shape, dt=F32):
        return nc.alloc_sbuf_tensor(name, list(shape), dt) if False else nc.sbuf_tensor(name, list(shape), dt)

    import contextlib
    es = contextlib.ExitStack()
    with es:
        def SB(name, shape, dt=F32):
            return es.enter_context(nc.sbuf_tensor(name, list(shape), dt))

        def PS(name, shape, dt=F32):
            return es.enter_context(nc.psum_tensor(name, list(shape), dt))

        cst = SB("cst", [128, NCONST])
        identb = SB("identb", [128, 128], BF16)
        causTs = cst[:, coff["causT"][0]:coff["causT"][0] + 128]
        ident = cst[:, coff["ident"][0]:coff["ident"][0] + 128]
        M1 = cst[:, coff["M1"][0]:coff["M1"][0] + 128]
        Mcol = cst[:, coff["Mcol"][0]:coff["Mcol"][0] + 4]
        gmaskT = cst[:, coff["gmaskT"][0]:coff["gmaskT"][0] + 128]
        eoff = cst[:, coff["eoff"][0]:coff["eoff"][0] + 32]
        LstrB = SB("LstrB", [128, 128], BF16)
        onesB = SB("onesB", [128, 128], BF16)
        gcol = SB("gcol", [128, 4 * KC])
        T1 = SB("T1", [128, KC, SBTOK], BF16)
        wb = [SB(f"wb{i}", [128, KC, 512], BF16) for i in range(2)]
        xin = SB("xin", [128, D])
        xsb = SB("xsb", [128, D], BF16)
        st = SB("st", [128, 16])
        fsb = SB("fsb", [128, 64])
        pz = [PS(f"pz{i}", [128, 512]) for i in range(2)]
        ptb = [PS(f"ptb{i}", [128, 8, 128], BF16) for i in range(2)]
        pss = [PS(f"pss{i}", [128, 512]) for i in range(2)]
        psb = [PS(f"psb{i}", [128, 512]) for i in range(2)]

        cnt = dict(z=0, t=0, s=0, b=0, w=0)

        def rot(kind, arr):
            i = cnt[kind] % len(arr)
            cnt[kind] += 1
            return i

        breg = {}

        def bound_reg(e):
            if not KREG:
                return NE * CAP - 1
            if "r" not in breg:
                breg["r"] = e.alloc_register("bndreg")
            e.reg_mov(breg["r"], NE * CAP - 1)
            return breg["r"]

        def dma(dst, src, r=(), w=(), q="sp", twice=False):
            S.add(q, lambda e, d=dst, s_=src: e.dma_start(out=d, in_=s_), r=r, w=w, kind="d")
            if twice:
                S.add(q, lambda e, d=dst, s_=src: e.dma_start(out=d, in_=s_), r=r, w=w, kind="d")

        def load_w(bi, pieces, r=("wfull",)):
            for (src2d, kc_n, co, n) in pieces:
                dst = wb[bi][:, 0:kc_n, co:co + n]
                src = src2d.rearrange("(c p) n -> p c n", p=128)
                dma(dst, src, r=r, w=(("wb", bi),), q="pool")

        def mm_group(ps_ap, T, tcol0, bi, ncols, kc_n=None, wcol0=0):
            kc_n = kc_n or KC

            def fn(e):
                ins = None
                for kc in range(kc_n):
                    ins = e.matmul(ps_ap, lhsT=T[:, kc, tcol0:tcol0 + 128], rhs=wb[bi][:, kc, wcol0:wcol0 + ncols],
                                   start=(kc == 0), stop=(kc == kc_n - 1))
                return ins
            return fn

        def mm_group_T(ps_ap, bi, wcol0, T, tcol0, ntok, kc_n=None):
            kc_n = kc_n or KC

            def fn(e):
                ins = None
                for kc in range(kc_n):
                    ins = e.matmul(ps_ap, lhsT=wb[bi][:, kc, wcol0:wcol0 + 128], rhs=T[:, kc, tcol0:tcol0 + ntok],
                                   start=(kc == 0), stop=(kc == kc_n - 1))
                return ins
            return fn

        def rstd_ops(src_ap, n, ss, rs, key, junk_ap, junk_key):
            S.add("act", lambda e: e.activation(out=junk_ap, in_=src_ap, func=AF.Square, accum_out=ss),
                  r=(key,), w=(junk_key, ("st", "ss")))
            S.add("act", lambda e: e.activation(out=rs, in_=ss, func=AF.Sqrt, scale=1.0 / n, bias=EPS),
                  r=(("st", "ss"),), w=(("st", "rs0"),))
            S.add("dve", lambda e: e.reciprocal(out=rs, in_=rs), r=(("st", "rs0"),), w=(("st", "rs"),))

        def transpose_to_T(src_bf, T, tcol0, gc0, srckey, tkey, nchunks=None):
            nchunks = nchunks or KC
            for c0 in range(0, nchunks, 7):
                n = min(7, nchunks - c0)
                pi = rot("t", ptb)

                def fn(e, c0=c0, n=n, pi=pi):
                    for j in range(n):
                        e.transpose(ptb[pi][:, j, :], src_bf[:, (c0 + j) * 128:(c0 + j + 1) * 128], identb[:])
                    e.transpose(ptb[pi][:, 7, :], src_bf[:, 0:128], identb[:])
                    return e.transpose(ptb[pi][:, 7, :], src_bf[:, 0:128], identb[:])
                S.add("pe", fn, r=(srckey, "identb"), w=(("ptb", pi),))
                for j in range(n):
                    kc = c0 + j
                    if gc0 is None:
                        if j % 2 == 0:
                            S.add("act", lambda e, pi=pi, j=j, kc=kc: e.activation(
                                out=T[:, kc, tcol0:tcol0 + 128], in_=ptb[pi][:, j, :], func=AF.Copy),
                                r=(("ptb", pi),), w=(tkey,))
                        else:
                            S.add("dve", lambda e, pi=pi, j=j, kc=kc: e.tensor_copy(
                                out=T[:, kc, tcol0:tcol0 + 128], in_=ptb[pi][:, j, :]),
                                r=(("ptb", pi),), w=(tkey,))
                    else:
                        if j % 2 == 0:
                            S.add("act", lambda e, pi=pi, j=j, kc=kc: e.activation(
                                out=T[:, kc, tcol0:tcol0 + 128], in_=ptb[pi][:, j, :], func=AF.Copy,
                                scale=gcol[:, gc0 + kc:gc0 + kc + 1]), r=(("ptb", pi), "gcol"), w=(tkey,))
                        else:
                            S.add("dve", lambda e, pi=pi, j=j, kc=kc: e.tensor_scalar(
                                out=T[:, kc, tcol0:tcol0 + 128], in0=ptb[pi][:, j, :],
                                scalar1=gcol[:, gc0 + kc:gc0 + kc + 1], scalar2=None, op0=ALU.mult),
                                r=(("ptb", pi), "gcol"), w=(tkey,))

        def norm_tile_to_T(src_dram_rows, T, tcol0, gc0, tkey):
            dma(xin[:], src_dram_rows, r=("dram_in",), w=("xin",))
            dma(xin[:], src_dram_rows, r=("dram_in",), w=("xin",))
            rstd_ops(xin[:], D, st[:, 0:1], st[:, 1:2], "xin", xsb[:], "xsb")
            S.add("act", lambda e: e.activation(out=xsb[:], in_=xin[:], func=AF.Copy, scale=st[:, 1:2]),
                  r=("xin", ("st", "rs")), w=("xsb",))
            transpose_to_T(xsb, T, tcol0, gc0, "xsb", tkey)

        dma(cst[:], consts.ap(), w=("cst",))
        dma(gcol[:], gcols.ap(), w=("gcol",))
        S.add("dve", lambda e: e.tensor_copy(out=identb[:], in_=ident), r=("cst",), w=("identb",))
        S.add("dve", lambda e: e.tensor_copy(out=LstrB[:], in_=cst[:, coff["Lstrict"][0]:coff["Lstrict"][0] + 128]),
              r=("cst",), w=("LstrB",))
        S.add("dve", lambda e: e.tensor_copy(out=onesB[:], in_=cst[:, coff["ones"][0]:coff["ones"][0] + 128]),
              r=("cst",), w=("onesB",))
        for k in (wshape if GATHER else ()):
            rows = wshape[k][0] // NCORES
            step = max(1, min(rows, (1 << 25) // (wshape[k][1] * 4)))
            if k in ("w1", "w3", "w2"):
                rg = rows // 4
                for g in range(4):
                    dma(wbn[k][g][:, :], wsh[k][g * rg:(g + 1) * rg, :], w=(("wbn", k, g),), q="sp")
                continue
            for r0 in range(0, rows, step):
                r1 = min(rows, r0 + step)
                dma(wbn[k][r0:r1, :], wsh[k][r0:r1, :], w=(("wbn", k),), q="sp")
        S.flush()
        if KSTOP == 1:
            return nc
        ncc = [0]
        for k in (wshape if GATHER else ()):
            if k in wfe:
                rows = wshape[k][0] // 32
                for g in range(4):
                    ncc[0] += 1
                    if ncc[0] > KNCC:
                        continue
                    S.add("pool", lambda e, k=k, g=g, rows=rows: e.collective_compute(
                        "AllGather", ALU.bypass, replica_groups=[list(range(NCORES))],
                        ins=[wbn[k][g].ap()], outs=[wfe[k][g].ap()]), w=("wfull",), kind="cc")
            else:
                ncc[0] += 1
                if ncc[0] > KNCC:
                    continue
                S.add("pool", lambda e, k=k: e.collective_compute(
                    "AllGather", ALU.bypass, replica_groups=[list(range(NCORES))],
                    ins=[wbn[k].ap()], outs=[wfull[k].ap()]), w=("wfull",), kind="cc")
        S.flush()
        if KSTOP == 2:
            return nc

        W_in, W_out = wfull["w_in"], wfull["w_out"]

        with contextlib.ExitStack() as ea:
            def SA(name, shape, dt=F32):
                return ea.enter_context(nc.sbuf_tensor(name, list(shape), dt))
            lnps = SA("lnps", [128, 5 * DH])
            lng, lnb = lnps[:, 0:DH], lnps[:, DH:2 * DH]
            lbv, omlv = lnps[:, 2 * DH:3 * DH], lnps[:, 3 * DH:4 * DH]
            hng = lnps[:, 4 * DH:5 * DH]
            wsTs = SA("wsTs", [128, GH * 128])
            wsTb = SA("wsTb", [128, GH * 128], BF16)
            bscs = SA("bscs", [128, GH])
            Sst = SA("Sst", [128, HH, 128])
            sbuf_s = [SA(f"sbuf_s{i}", [128, 512]) for i in range(SBT)]
            gv = SA("gv", [128, 512])
            vn = SA("vn", [128, 512])
            vln = SA("vln", [128, 512], BF16)
            ya = SA("ya", [128, 512], BF16)
            NTS = 2
            hn_names = ["sig", "fg", "logf", "kh", "e1", "e2", "sq", "of", "sg", "tmp"]
            ht = [{n: SA(f"h_{n}{i}", [128, 128]) for n in hn_names} for i in range(NTS)]
            hb = [{n: SA(f"hb_{n}{i}", [128, 128], BF16) for n in ["qt", "kt", "qtT", "ktT", "aTm", "Stl", "vh"]}
                  for i in range(NTS)]
            hs = [SA(f"hs{i}", [128, 16]) for i in range(NTS)]
            yb2 = [SA(f"yb2_{i}", [128, 256], BF16) for i in range(SBT)]

            dma(lnps[:], lnp.ap(), w=("lnps",))
            dma(wsTs[:], wsT.ap(), w=("wsTs",))
            dma(bscs[:], bsc.ap(), w=("bscs",))
            S.add("dve", lambda e: e.tensor_tensor(out=lbv, in0=lbv, in1=omlv, op=ALU.subtract), r=("lnps",), w=("lnps",))
            S.add("act", lambda e: e.activation(out=lbv, in_=lbv, func=AF.Sigmoid), r=("lnps",), w=("lnps",))
            S.add("dve", lambda e: e.tensor_scalar(out=omlv, in0=lbv, scalar1=-1.0, scalar2=1.0, op0=ALU.mult, op1=ALU.add),
                  r=("lnps",), w=("lnps",))
            for h in range(GH):
                S.add("dve", lambda e, h=h: e.tensor_tensor(out=wsTb[:, h * 128:(h + 1) * 128], in0=wsTs[:, h * 128:(h + 1) * 128],
                                                            in1=gmaskT, op=ALU.mult), r=("wsTs", "cst"), w=("wsTb",))
            S.add("dve", lambda e: e.memset(Sst[:], 0.0), w=tuple(("S", h) for h in range(HH)))

            def hgrn_head_tile(zi, h, full, row0):
                p = rot("s", [0] * NTS)
                t_, b_, s_ = ht[p], hb[p], hs[p]
                K = lambda n: ("h", n, p)
                if full:
                    cq, cf, ci, cg = 0, 128, 256, 384
                else:
                    cf, ci = full_cols[0], full_cols[1]
                z = pz[zi]
                zk = ("pz", zi)
                si = rot("b", pss)
                pS = pss[si]
                pk = ("pss", si)
                bi_ = rot("w", psb)
                pB = psb[bi_]
                pbk = ("psb", bi_)
                lbh, omlh = lbv[:, h * 128:(h + 1) * 128], omlv[:, h * 128:(h + 1) * 128]
                S.add("act", lambda e: e.activation(out=t_["sig"][:], in_=z[:, cf:cf + 128], func=AF.Sigmoid),
                      r=(zk,), w=(K("sig"),))
                S.add("dve", lambda e: e.tensor_tensor(out=t_["fg"][:], in0=t_["sig"][:], in1=omlh, op=ALU.mult),
                      r=(K("sig"), "lnps"), w=(K("fg"),))
                S.add("dve", lambda e: e.tensor_tensor(out=t_["fg"][:], in0=t_["fg"][:], in1=lbh, op=ALU.add),
                      r=(K("fg"), "lnps"), w=(K("fg"),))
                S.add("act", lambda e: e.activation(out=t_["logf"][:], in_=t_["fg"][:], func=AF.Ln),
                      r=(K("fg"),), w=(K("logf"),))
                S.add("dve", lambda e: e.tensor_scalar(out=t_["kh"][:], in0=t_["fg"][:], scalar1=-1.0, scalar2=1.0,
                                                       op0=ALU.mult, op1=ALU.add), r=(K("fg"),), w=(K("kh"),))
                S.add("pe", lambda e: e.matmul(pS[:, 0:128], lhsT=M1, rhs=t_["logf"][:], start=True, stop=True),
                      r=(K("logf"), "cst"), w=(pk,))
                S.add("pe", lambda e: e.matmul(pB[:, 0:4], lhsT=t_["logf"][:], rhs=Mcol, start=True, stop=True),
                      r=(K("logf"), "cst"), w=(pbk,))
                S.add("act", lambda e: e.activation(out=t_["e2"][:], in_=pS[:, 0:128], func=AF.Exp, scale=-1.0),
                      r=(pk,), w=(K("e2"),))
                if full:
                    S.add("act", lambda e: e.activation(out=t_["e1"][:], in_=pS[:, 0:128], func=AF.Exp),
                          r=(pk,), w=(K("e1"),))
                S.add("act", lambda e: e.activation(out=s_[:, 0:4], in_=pB[:, 0:4], func=AF.Exp),
                      r=(pbk,), w=(K("cc"),))
                S.add("dve", lambda e: e.tensor_tensor(out=b_["kt"][:], in0=t_["kh"][:], in1=t_["e2"][:], op=ALU.mult),
                      r=(K("kh"), K("e2")), w=(K("kt"),))
                S.add("act", lambda e: e.activation(out=b_["vh"][:], in_=z[:, ci:ci + 128], func=AF.Copy),
                      r=(zk,), w=(K("vh"),))
                if full:
                    S.add("act", lambda e: e.activation(out=t_["sq"][:], in_=z[:, cq:cq + 128], func=AF.Silu),
                          r=(zk,), w=(K("sq"),))
                    S.add("dve", lambda e: e.tensor_tensor(out=b_["qt"][:], in0=t_["sq"][:], in1=t_["e1"][:], op=ALU.mult),
                          r=(K("sq"), K("e1")), w=(K("qt"),))
                    S.add("act", lambda e: e.activation(out=t_["sg"][:], in_=z[:, cg:cg + 128], func=AF.Silu),
                          r=(zk,), w=(K("sg"),))
                    pi = rot("t", ptb)

                    def tq(e):
                        e.transpose(ptb[pi][:, 0, :], b_["qt"][:], identb[:])
                        e.transpose(ptb[pi][:, 1, :], b_["kt"][:], identb[:])
                        e.transpose(ptb[pi][:, 7, :], b_["kt"][:], identb[:])
                        return e.transpose(ptb[pi][:, 7, :], b_["kt"][:], identb[:])
                    S.add("pe", tq, r=(K("qt"), K("kt"), "identb"), w=(("ptb", pi),))
                    S.add("act", lambda e: e.activation(out=b_["qtT"][:], in_=ptb[pi][:, 0, :], func=AF.Copy),
                          r=(("ptb", pi),), w=(K("qtT"),))
                    S.add("dve", lambda e: e.tensor_copy(out=b_["ktT"][:], in_=ptb[pi][:, 1, :]),
                          r=(("ptb", pi),), w=(K("ktT"),))
                    S.add("pe", lambda e: e.matmul(pS[:, 128:256], lhsT=b_["ktT"][:], rhs=b_["qtT"][:], start=True, stop=True),
                          r=(K("ktT"), K("qtT")), w=((pk, "a"),))
                    S.add("dve", lambda e: e.tensor_tensor(out=b_["aTm"][:], in0=pS[:, 128:256], in1=causTs, op=ALU.mult),
                          r=((pk, "a"), "cst"), w=(K("aTm"),))
                    S.add("dve", lambda e: e.tensor_scalar(out=b_["Stl"][:], in0=Sst[:, h, :], scalar1=s_[:, 0:1], scalar2=None,
                                                           op0=ALU.mult), r=(("S", h), K("cc")), w=(K("Stl"),))

                    def ofn(e):
                        e.matmul(pS[:, 256:384], lhsT=b_["aTm"][:], rhs=b_["vh"][:], start=True, stop=False)
                        return e.matmul(pS[:, 256:384], lhsT=b_["qtT"][:], rhs=b_["Stl"][:], start=False, stop=True)
                    S.add("pe", ofn, r=(K("aTm"), K("vh"), K("qtT"), K("Stl")), w=((pk, "o"),))
                S.add("pe", lambda e: e.matmul(pS[:, 384:512], lhsT=b_["kt"][:], rhs=b_["vh"][:], start=True, stop=True),
                      r=(K("kt"), K("vh")), w=((pk, "i"),))
                S.add("dve", lambda e: e.tensor_scalar(out=t_["tmp"][:], in0=pS[:, 384:512], scalar1=s_[:, 2:3], scalar2=None,
                                                       op0=ALU.mult), r=((pk, "i"), K("cc")), w=(K("tmp"),))
                stl_dep = (K("Stl"),) if full else ()
                S.add("dve", lambda e: e.scalar_tensor_tensor(out=Sst[:, h, :], in0=Sst[:, h, :], scalar=s_[:, 1:2],
                                                              in1=t_["tmp"][:], op0=ALU.mult, op1=ALU.add),
                      r=(K("tmp"), K("cc")) + stl_dep, w=(("S", h),))
                if full:
                    S.add("act", lambda e: e.activation(out=t_["of"][:], in_=pS[:, 256:384], func=AF.Copy),
                          r=((pk, "o"),), w=(K("of"),))
                    S.add("act", lambda e: e.activation(out=t_["tmp"][:], in_=t_["of"][:], func=AF.Square, accum_out=s_[:, 4:5]),
                          r=(K("of"),), w=(K("tmp"), K("ss")))
                    S.add("act", lambda e: e.activation(out=s_[:, 5:6], in_=s_[:, 4:5], func=AF.Sqrt, scale=1.0 / 128, bias=EPS),
                          r=(K("ss"),), w=(K("rs0"),))
                    S.add("dve", lambda e: e.reciprocal(out=s_[:, 6:7], in_=s_[:, 5:6]), r=(K("rs0"),), w=(K("rs"),))
                    S.add("dve", lambda e: e.scalar_tensor_tensor(out=t_["of"][:], in0=t_["of"][:], scalar=s_[:, 6:7],
                                                                  in1=hng[:, h * 128:(h + 1) * 128], op0=ALU.mult, op1=ALU.mult),
                          r=(K("of"), K("rs"), "lnps"), w=(K("of"),))
                    tl_ = (row0 // 128) % SBT
                    ybp = yb2[tl_]
                    S.add("dve", lambda e: e.tensor_tensor(out=ybp[:, (h % 2) * 128:(h % 2 + 1) * 128], in0=t_["of"][:], in1=t_["sg"][:],
                                                           op=ALU.mult), r=(K("of"), K("sg")), w=(("yb2", tl_, h % 2),))
                    if h % 2 == 1:
                        dma(y_d[row0:row0 + 128, DH + (h - 1) * 128:DH + (h + 1) * 128], ybp[:],
                            r=(("yb2", tl_, 0), ("yb2", tl_, 1)), w=("y_d",))

            full_cols = [0, 0]
            wcnt = [0]

            def next_wb():
                i = wcnt[0] % 2
                wcnt[0] += 1
                return i

            for sbi in range(0 if 'p' in KSKIP else NSB):
                for tl in range(SBT):
                    r0 = (sbi * SBT + tl) * 128
                    norm_tile_to_T(x_pre[r0:r0 + 128, :], T1, tl * 128, 0, ("T1", tl))
                for u in range(HH // 2):
                    bi = next_wb()
                    pieces = []
                    for j in range(2):
                        h = 2 * u + j
                        pieces.append((W_in[:, 3 * DH + h * 128:3 * DH + (h + 1) * 128], KC, j * 256, 128))
                        pieces.append((W_in[:, 4 * DH + h * 128:4 * DH + (h + 1) * 128], KC, j * 256 + 128, 128))
                    load_w(bi, pieces)
                    for tl in range(SBT):
                        zi = rot("z", pz)
                        S.add("pe", mm_group(pz[zi][:, :], T1, tl * 128, bi, 512), r=(("T1", tl), ("wb", bi)), w=(("pz", zi),))
                        for j in range(2):
                            full_cols[0], full_cols[1] = j * 256, j * 256 + 128
                            hgrn_head_tile(zi, 2 * u + j, False, 0)
            for sbi in range(NSB):
                for tl in range(SBT):
                    r0 = (sbi * SBT + tl) * 128
                    norm_tile_to_T(x_own[r0:r0 + 128, :], T1, tl * 128, 0, ("T1", tl))
                for h in range(0 if 'g' in KSKIP else GH):
                    bi = next_wb()
                    load_w(bi, [(W_in[:, DH + h * 512:DH + (h + 1) * 512], KC, 0, 512)])
                    for tl in range(SBT):
                        zi = rot("z", pz)
                        z = pz[zi]
                        zk = ("pz", zi)
                        S.add("pe", mm_group(z[:, :], T1, tl * 128, bi, 512), r=(("T1", tl), ("wb", bi)), w=(zk,))
                        S.add("act", lambda e, z=z: e.activation(out=gv[:], in_=z[:, :], func=AF.Gelu, accum_out=st[:, 2:3]),
                              r=(zk,), w=("gv", ("st", "s1")))
                        S.add("act", lambda e: e.activation(out=vn[:], in_=gv[:], func=AF.Square, accum_out=st[:, 3:4]),
                              r=("gv",), w=("vn", ("st", "s2")))
                        S.add("dve", lambda e: e.tensor_scalar(out=st[:, 4:5], in0=st[:, 2:3], scalar1=1.0 / 512, scalar2=None,
                                                               op0=ALU.mult), r=(("st", "s1"),), w=(("st", "mean"),))
                        S.add("dve", lambda e: e.tensor_tensor(out=st[:, 5:6], in0=st[:, 4:5], in1=st[:, 4:5], op=ALU.mult),
                              r=(("st", "mean"),), w=(("st", "msq"),))
                        S.add("dve", lambda e: e.scalar_tensor_tensor(out=st[:, 6:7], in0=st[:, 3:4], scalar=1.0 / 512,
                                                                      in1=st[:, 5:6], op0=ALU.mult, op1=ALU.subtract),
                              r=(("st", "s2"), ("st", "msq")), w=(("st", "var"),))
                        S.add("act", lambda e: e.activation(out=st[:, 7:8], in_=st[:, 6:7], func=AF.Sqrt, bias=EPS),
                              r=(("st", "var"),), w=(("st", "sd"),))
                        S.add("dve", lambda e: e.reciprocal(out=st[:, 8:9], in_=st[:, 7:8]), r=(("st", "sd"),), w=(("st", "lrs"),))
                        S.add("dve", lambda e: e.scalar_tensor_tensor(out=st[:, 9:10], in0=st[:, 4:5], scalar=-1.0,
                                                                      in1=st[:, 8:9], op0=ALU.mult, op1=ALU.mult),
                              r=(("st", "mean"), ("st", "lrs")), w=(("st", "nmr"),))
                        S.add("act", lambda e: e.activation(out=vn[:], in_=gv[:], func=AF.Identity, scale=st[:, 8:9],
                                                            bias=st[:, 9:10]), r=("gv", ("st", "lrs"), ("st", "nmr")), w=("vn",))
                        S.add("dve", lambda e, h=h: e.tensor_tensor(out=vn[:], in0=vn[:], in1=lng[:, h * 512:(h + 1) * 512],
                                                                    op=ALU.mult), r=("vn", "lnps"), w=("vn",))
                        S.add("dve", lambda e, h=h: e.tensor_tensor(out=vln[:], in0=vn[:], in1=lnb[:, h * 512:(h + 1) * 512],
                                                                    op=ALU.add), r=("vn", "lnps"), w=("vln",))
                        si = rot("b", pss)
                        S.add("pe", lambda e, si=si, h=h: e.matmul(pss[si][:, :], lhsT=wsTb[:, h * 128:(h + 1) * 128], rhs=vln[:],
                                                                   start=True, stop=True), r=("vln", "wsTb"),
                              w=(("pss", si), (("pss", si), "a"), (("pss", si), "o"), (("pss", si), "i")))
                        S.add("act", lambda e, si=si, h=h, tl=tl: e.activation(out=sbuf_s[tl][:], in_=pss[si][:, :], func=AF.Identity,
                                                                               bias=bscs[:, h:h + 1]),
                              r=(("pss", si), "bscs"), w=(("sbuf_s", tl),))
                    bi = next_wb()
                    load_w(bi, [(W_in[:, h * 512:(h + 1) * 512], KC, 0, 512)])
                    for tl in range(SBT):
                        r0 = (sbi * SBT + tl) * 128
                        zi = rot("z", pz)
                        z = pz[zi]
                        zk = ("pz", zi)
                        S.add("pe", mm_group(z[:, :], T1, tl * 128, bi, 512), r=(("T1", tl), ("wb", bi)), w=(zk,))
                        S.add("act", lambda e, z=z: e.activation(out=gv[:], in_=z[:, :], func=AF.Gelu), r=(zk,), w=("gv",))
                        S.add("dve", lambda e, tl=tl: e.tensor_tensor(out=ya[:], in0=gv[:], in1=sbuf_s[tl][:], op=ALU.mult),
                              r=("gv", ("sbuf_s", tl)), w=("ya",))
                        if 'y' not in KSKIP:
                            dma(y_d[r0:r0 + 128, h * 512:(h + 1) * 512], ya[:], r=("ya",), w=("y_d",))
                for h in range(0 if 'h' in KSKIP else HH):
                    bi = next_wb()
                    load_w(bi, [(W_in[:, (2 + j) * DH + h * 128:(2 + j) * DH + (h + 1) * 128], KC, j * 128, 128) for j in range(4)])
                    for tl in range(SBT):
                        r0 = (sbi * SBT + tl) * 128
                        zi = rot("z", pz)
                        S.add("pe", mm_group(pz[zi][:, :], T1, tl * 128, bi, 512), r=(("T1", tl), ("wb", bi)), w=(("pz", zi),))
                        hgrn_head_tile(zi, h, True, r0)
            S.flush()
            if KSTOP == 3:
                return nc

        def linear_residual(Wd, res_d, out_d, sbi, xr, ho):
            for u in range(D // 512):
                bi = next_wb()
                load_w(bi, [(Wd[:, u * 512:(u + 1) * 512], KC, 0, 512)])
                for tl in range(SBT):
                    r0 = (sbi * SBT + tl) * 128
                    zi = rot("z", pz)
                    S.add("pe", mm_group(pz[zi][:, :], T1, tl * 128, bi, 512), r=(("T1", tl), ("wb", bi)), w=(("pz", zi),))
                    dma(xr[:], res_d[r0:r0 + 128, u * 512:(u + 1) * 512], r=("res_d",), w=("xr",), twice=True)
                    S.add("dve", lambda e, zi=zi: e.tensor_tensor(out=ho[:], in0=pz[zi][:, :], in1=xr[:], op=ALU.add),
                          r=(("pz", zi), "xr"), w=("ho",))
                    dma(out_d[r0:r0 + 128, u * 512:(u + 1) * 512], ho[:], r=("ho",), w=("out_d",))

        with contextlib.ExitStack() as eb:
            xr = eb.enter_context(nc.sbuf_tensor("xr", [128, 512], F32))
            ho = eb.enter_context(nc.sbuf_tensor("ho", [128, 512], F32))
            for sbi in range(NSB):
                for tl in range(SBT):
                    r0 = (sbi * SBT + tl) * 128
                    dma(xsb[:], y_d[r0:r0 + 128, :], r=("y_d",), w=("xsb",), twice=True)
                    transpose_to_T(xsb, T1, tl * 128, None, "xsb", ("T1", tl))
                linear_residual(W_out, x_own, h1_d, sbi, xr, ho)
            S.flush()
            if KSTOP == 4:
                return nc

            with contextlib.ExitStack() as ecx:
                def SC(name, shape, dt=F32):
                    return ecx.enter_context(nc.sbuf_tensor(name, list(shape), dt))
                T2 = SC("T2", [128, KC, SBTOK], BF16)
                TM = T2
                KT = SC("KT", [128, KC, NMEM], BF16)
                Vs = SC("Vs", [128, NMEM // 128, D], BF16)
                pf = SC("pf", [128, NMEM])
                pn = SC("pn", [128, NMEM], BF16)
                pT = SC("pT", [128, NMEM // 128, 128], BF16)
                MT = NMEM // 128
                for mt in range(MT):
                    norm_tile_to_T(mem[mt * 128:(mt + 1) * 128, :], TM, mt * 128, 2 * KC, "T2")
                TMk = ("T2",)
                for u in range(D // 512):
                    bi = next_wb()
                    load_w(bi, [(wfull["w_k"][:, u * 512:(u + 1) * 512], KC, 0, 512)])
                    for j in range(4):
                        zi = rot("z", pz)
                        S.add("pe", mm_group_T(pz[zi][:, 0:NMEM], bi, j * 128, TM, 0, NMEM), r=TMk + (("wb", bi),), w=(("pz", zi),))
                        S.add("act", lambda e, zi=zi, fc=u * 4 + j: e.activation(out=KT[:, fc, :], in_=pz[zi][:, 0:NMEM], func=AF.Copy),
                              r=(("pz", zi),), w=("KT",))
                for u in range(D // 512):
                    bi = next_wb()
                    load_w(bi, [(wfull["w_v"][:, u * 512:(u + 1) * 512], KC, 0, 512)])
                    for mt in range(MT):
                        zi = rot("z", pz)
                        S.add("pe", mm_group(pz[zi][:, :], TM, mt * 128, bi, 512), r=TMk + (("wb", bi),), w=(("pz", zi),))
                        S.add("act", lambda e, zi=zi, mt=mt, u=u: e.activation(out=Vs[:, mt, u * 512:(u + 1) * 512], in_=pz[zi][:, :],
                                                                               func=AF.Copy), r=(("pz", zi),), w=("Vs",))
                T1all = tuple(("T1", tl) for tl in range(SBT))
                xscale = float((D // XH) ** -0.5)
                for sbi in range(NSB):
                    for tl in range(SBT):
                        r0 = (sbi * SBT + tl) * 128
                        norm_tile_to_T(h1_d[r0:r0 + 128, :], T1, tl * 128, KC, ("T1", tl))
                    for u in range(D // 512):
                        bi = next_wb()
                        load_w(bi, [(wfull["w_q"][:, u * 512:(u + 1) * 512], KC, 0, 512)])
                        for j in range(4):
                            zi = rot("z", pz)
                            S.add("pe", mm_group_T(pz[zi][:, 0:SBTOK], bi, j * 128, T1, 0, SBTOK), r=T1all + (("wb", bi),),
                                  w=(("pz", zi),))
                            S.add("act", lambda e, zi=zi, fc=u * 4 + j: e.activation(out=T2[:, fc, :], in_=pz[zi][:, 0:SBTOK],
                                                                                     func=AF.Copy, scale=xscale),
                                  r=(("pz", zi),), w=("T2",))
                    for tl in range(SBT):
                        for h in range(XH):
                            si = rot("b", pss)
                            pS = pss[si]

                            def sfn(e, pS=pS, h=h, tl=tl):
                                ins = None
                                for c in range(XKC):
                                    ins = e.matmul(pS[:, 0:NMEM], lhsT=T2[:, h * XKC + c, tl * 128:(tl + 1) * 128],
                                                   rhs=KT[:, h * XKC + c, :], start=(c == 0), stop=(c == XKC - 1))
                                return ins
                            S.add("pe", sfn, r=("T2", "KT"), w=(("pss", si),))
                            S.add("dve", lambda e, pS=pS: e.reduce_max(out=st[:, 10:11], in_=pS[:, 0:NMEM], axis=AX.X),
                                  r=(("pss", si),), w=(("st", "mx"),))
                            S.add("dve", lambda e: e.tensor_scalar(out=st[:, 11:12], in0=st[:, 10:11], scalar1=-1.0, scalar2=None,
                                                                   op0=ALU.mult), r=(("st", "mx"),), w=(("st", "nmx"),))
                            S.add("act", lambda e, pS=pS: e.activation(out=pf[:], in_=pS[:, 0:NMEM], func=AF.Exp, bias=st[:, 11:12],
                                                                       accum_out=st[:, 12:13]),
                                  r=(("pss", si), ("st", "nmx")), w=("pf", ("st", "sm")))
                            S.add("dve", lambda e: e.reciprocal(out=st[:, 13:14], in_=st[:, 12:13]), r=(("st", "sm"),), w=(("st", "rsm"),))
                            S.add("dve", lambda e: e.tensor_scalar(out=pn[:], in0=pf[:], scalar1=st[:, 13:14], scalar2=None, op0=ALU.mult),
                                  r=("pf", ("st", "rsm")), w=("pn",))
                            pi = rot("t", ptb)

                            def tfn(e, pi=pi):
                                ins = None
                                for mc in range(MT):
                                    ins = e.transpose(ptb[pi][:, mc, :], pn[:, mc * 128:(mc + 1) * 128], identb[:])
                                e.transpose(ptb[pi][:, 7, :], pn[:, 0:128], identb[:])
                                return e.transpose(ptb[pi][:, 7, :], pn[:, 0:128], identb[:])
                            S.add("pe", tfn, r=("pn", "identb"), w=(("ptb", pi),))
                            S.add("act", lambda e, pi=pi: e.activation(out=pT[:], in_=ptb[pi][:, 0:MT, :], func=AF.Copy),
                                  r=(("ptb", pi),), w=("pT",))
                            for c0 in range(0, XKC, 4):
                                n = min(4, XKC - c0)
                                zi = rot("z", pz)

                                def ofn2(e, zi=zi, c0=c0, n=n, h=h):
                                    ins = None
                                    for c in range(n):
                                        for mc in range(MT):
                                            col = (h * XKC + c0 + c) * 128
                                            ins = e.matmul(pz[zi][:, c * 128:(c + 1) * 128], lhsT=Vs[:, mc, col:col + 128],
                                                           rhs=pT[:, mc, :], start=(mc == 0), stop=(mc == MT - 1))
                                    return ins
                                S.add("pe", ofn2, r=("Vs", "pT"), w=(("pz", zi),))
                                S.add("dve", lambda e, zi=zi, c0=c0, n=n, h=h, tl=tl: e.tensor_copy(
                                    out=T1[:, h * XKC + c0:h * XKC + c0 + n, tl * 128:(tl + 1) * 128],
                                    in_=pz[zi][:, 0:n * 128].rearrange("p (c t) -> p c t", c=n)),
                                    r=(("pz", zi),), w=(("T1", tl),))
                    linear_residual(wfull["w_o"], h1_d, h2_d, sbi, xr, ho)
                S.flush()
                if KSTOP == 5:
                    return nc

        with contextlib.ExitStack() as ed:
            def SD(name, shape, dt=F32):
                return ed.enter_context(nc.sbuf_tensor(name, list(shape), dt))
            gates = SD("gates", [128, NT, 2])
            idxs = SD("idxs", [128, NT, 2], I32)
            er = contextlib.ExitStack()

            def SR(name, shape, dt=F32):
                return er.enter_context(nc.sbuf_tensor(name, list(shape), dt))
            nmb = SR("nmb", [128, D])
            xnf = SR("xnf", [128, D])
            xT32 = SR("xT32", [128, KC, 128])
            wr = SR("wr_s", [128, KC, 36])
            brs = SR("brs", [128, 36])
            Mall = SR("Mall", [128, NT, 32], BF16)
            rt = SR("rt", [128, 256])
            dma(nmb[:], nmoe_b.ap(), w=("nmb",))
            dma(wr[:], wr_in.ap().rearrange("(c p) n -> p c n", p=128), w=("wr",))
            dma(brs[:], br_in.ap(), w=("brs",))
            S.add("dve", lambda e: e.memset(xsb[:], 0.0), w=("xsb",))
            for blk in range(NE * CAP // 128):
                dma(xg_d[blk * 128:(blk + 1) * 128, :], xsb[:], r=("xsb",), w=(("xg_d", "z", blk),))
            S.flush()
            if KSTOP == 6:
                return nc
            for t in range(NT):
                r0 = t * 128
                dma(xin[:], h2_d[r0:r0 + 128, :], w=("xin",), twice=True)
                rstd_ops(xin[:], D, st[:, 0:1], st[:, 1:2], "xin", xsb[:], "xsb")
                S.add("dve", lambda e: e.scalar_tensor_tensor(out=xnf[:], in0=xin[:], scalar=st[:, 1:2], in1=nmb[:],
                                                              op0=ALU.mult, op1=ALU.mult), r=("xin", ("st", "rs"), "nmb"), w=("xnf",))
                S.add("act", lambda e: e.activation(out=xsb[:], in_=xnf[:], func=AF.Copy), r=("xnf",), w=("xsb",))
                for c0 in range(0, KC, 3):
                    n = min(3, KC - c0)
                    zi = rot("z", pz)

                    def tf(e, zi=zi, c0=c0, n=n):
                        for j in range(n):
                            e.transpose(pz[zi][:, j * 128:(j + 1) * 128], xnf[:, (c0 + j) * 128:(c0 + j + 1) * 128], ident)
                        e.transpose(pz[zi][:, 384:512], xnf[:, 0:128], ident)
                        return e.transpose(pz[zi][:, 384:512], xnf[:, 0:128], ident)
                    S.add("pe", tf, r=("xnf", "cst"), w=(("pz", zi),))
                    S.add("act", lambda e, zi=zi, c0=c0, n=n: e.activation(
                        out=xT32[:, c0:c0 + n, :], in_=pz[zi][:, 0:n * 128].rearrange("p (c t) -> p c t", c=n), func=AF.Copy),
                        r=(("pz", zi),), w=("xT32",))
                si = rot("b", pss)
                pS = pss[si]

                def rfn(e, pS=pS):
                    ins = None
                    for kc in range(KC):
                        ins = e.matmul(pS[:, 0:36], lhsT=xT32[:, kc, :], rhs=wr[:, kc, :], start=(kc == 0), stop=(kc == KC - 1))
                    return ins
                S.add("pe", rfn, r=("xT32", "wr"), w=(("pss", si),))
                lg = rt[:, 0:36]
                R = lambda a, b: rt[:, a:b]
                ops_ = []

                def V(fn, r=("rt",), w=("rt",)):
                    S.add("dve", fn, r=r, w=w)

                def A(fn, r=("rt",), w=("rt",)):
                    S.add("act", fn, r=r, w=w)
                V(lambda e, pS=pS: e.tensor_tensor(out=lg, in0=pS[:, 0:36], in1=brs[:], op=ALU.add), r=(("pss", si), "brs", "rt"))
                V(lambda e: e.reduce_max(out=R(40, 41), in_=R(0, 4), axis=AX.X))
                V(lambda e: e.tensor_scalar(out=R(44, 48), in0=R(0, 4), scalar1=R(40, 41), scalar2=None, op0=ALU.is_equal))
                V(lambda e: e.tensor_scalar(out=R(41, 42), in0=R(40, 41), scalar1=-1.0, scalar2=None, op0=ALU.mult))
                A(lambda e: e.activation(out=R(48, 52), in_=R(0, 4), func=AF.Exp, bias=R(41, 42), accum_out=R(42, 43)))
                V(lambda e: e.reciprocal(out=R(43, 44), in_=R(42, 43)))
                V(lambda e: e.tensor_scalar(out=R(56, 64), in0=R(4, 12), scalar1=R(44, 45), scalar2=None, op0=ALU.mult))
                for g in range(1, 4):
                    V(lambda e, g=g: e.scalar_tensor_tensor(out=R(56, 64), in0=R(4 + 8 * g, 12 + 8 * g), scalar=R(44 + g, 45 + g),
                                                            in1=R(56, 64), op0=ALU.mult, op1=ALU.add))
                V(lambda e: e.reduce_max(out=R(64, 65), in_=R(56, 64), axis=AX.X))
                V(lambda e: e.tensor_scalar(out=R(72, 80), in0=R(56, 64), scalar1=R(64, 65), scalar2=None, op0=ALU.is_equal))
                V(lambda e: e.scalar_tensor_tensor(out=R(80, 88), in0=R(72, 80), scalar=-1e30, in1=R(56, 64), op0=ALU.mult, op1=ALU.add))
                V(lambda e: e.reduce_max(out=R(65, 66), in_=R(80, 88), axis=AX.X))
                V(lambda e: e.tensor_scalar(out=R(88, 96), in0=R(80, 88), scalar1=R(65, 66), scalar2=None, op0=ALU.is_equal))
                V(lambda e: e.tensor_tensor(out=R(66, 67), in0=R(65, 66), in1=R(64, 65), op=ALU.subtract))
                A(lambda e: e.activation(out=R(67, 68), in_=R(66, 67), func=AF.Exp))
                V(lambda e: e.tensor_scalar(out=R(68, 69), in0=R(67, 68), scalar1=1.0, scalar2=None, op0=ALU.add))
                V(lambda e: e.reciprocal(out=R(69, 70), in_=R(68, 69)))
                V(lambda e, t=t: e.tensor_tensor(out=gates[:, t, 0:1], in0=R(69, 70), in1=R(43, 44), op=ALU.mult), w=("rt", "gates"))
                V(lambda e, t=t: e.tensor_tensor(out=gates[:, t, 1:2], in0=gates[:, t, 0:1], in1=R(67, 68), op=ALU.mult),
                  r=("rt", "gates"), w=("gates",))
                for g in range(4):
                    V(lambda e, g=g: e.tensor_scalar(out=R(96 + 8 * g, 104 + 8 * g), in0=R(72, 80), scalar1=R(44 + g, 45 + g),
                                                     scalar2=None, op0=ALU.mult))
                    V(lambda e, g=g: e.tensor_scalar(out=R(128 + 8 * g, 136 + 8 * g), in0=R(88, 96), scalar1=R(44 + g, 45 + g),
                                                     scalar2=None, op0=ALU.mult))
                V(lambda e, t=t: e.tensor_tensor(out=Mall[:, t, :], in0=R(96, 128), in1=R(128, 160), op=ALU.add), w=("rt", ("Mall", t)))
                bi_ = rot("w", psb)
                pB = psb[bi_]

                def pfn(e, pB=pB, t=t):
                    ins = None
                    for j in range(t + 1):
                        ins = e.matmul(pB[:, 0:32], lhsT=(LstrB[:] if j == t else onesB[:]), rhs=Mall[:, j, :],
                                       start=(j == 0), stop=(j == t))
                    return ins
                S.add("pe", pfn, r=tuple(("Mall", j) for j in range(t + 1)) + ("LstrB", "onesB"), w=(("psb", bi_),))
                V(lambda e, pB=pB: e.tensor_tensor(out=R(160, 192), in0=pB[:, 0:32], in1=eoff, op=ALU.add), r=(("psb", bi_), "cst", "rt"))
                for k in range(2):
                    V(lambda e, k=k: e.tensor_tensor(out=R(192, 224), in0=R(160, 192), in1=R(96 + 32 * k, 128 + 32 * k), op=ALU.mult))
                    V(lambda e, k=k: e.reduce_sum(out=R(224 + k, 225 + k), in_=R(192, 224), axis=AX.X))
                    V(lambda e, k=k, t=t: e.tensor_copy(out=idxs[:, t, k:k + 1], in_=R(224 + k, 225 + k)), w=("rt", ("idxs", t, k)))
                for k in ((0, 1, 0, 1) if KDUP else (0, 1)):
                    S.add("pool", lambda e, k=k, t=t: e.indirect_dma_start(
                        out=xg_d[:, :], out_offset=bass.IndirectOffsetOnAxis(ap=idxs[:, t, k:k + 1], axis=0),
                        in_=xsb[:], in_offset=None, bounds_check=bound_reg(e), oob_is_err=False),
                        r=("xsb", ("idxs", t, k)), w=(("xg_d", t, k),), kind="d")
                    if k == 1 and KFENCE:
                        dma(fence_d[:, 0:32], fsb[:, 0:32], r=("xsb",), w=(("xg_d", t, 0), ("xg_d", t, 1), "fence_d"), q="pool")
            S.flush()
            if KSTOP == 7:
                return nc
            er.close()

            with contextlib.ExitStack() as ee:
                def SE(name, shape, dt=F32):
                    return ee.enter_context(nc.sbuf_tensor(name, list(shape), dt))
                xgT = SE("xgT", [128, KC, CAP], BF16)
                hdT = SE("hdT", [128, FKC, CAP], BF16)
                s1 = SE("s1", [128, CAP])
                yo = [SE(f"yo{i}", [128, 512]) for i in range(2)]
                xgk = tuple(("xgT", tt) for tt in range(CT))
                w13 = [0]
                for ex in range(NE):
                    (W1, o1), (W3, o3), (W2, o2) = wexp("w1", ex), wexp("w3", ex), wexp("w2", ex)
                    for tt in range(CT):
                        r0 = ex * CAP + tt * 128
                        dma(xsb[:], xg_d[r0:r0 + 128, :], r=("xg_d",), w=("xsb",), twice=True)
                        transpose_to_T(xsb, xgT, tt * 128, None, "xsb", ("xgT", tt))
                    for u in range(FE // 256):
                        bi = w13[0] % 2
                        w13[0] += 1
                        load_w(bi, [(W1[o1:o1 + D, u * 256:(u + 1) * 256], KC, 0, 256),
                                    (W3[o3:o3 + D, u * 256:(u + 1) * 256], KC, 256, 256)])
                        for j in range(2):
                            fb = u * 2 + j
                            z1 = rot("z", pz)
                            S.add("pe", mm_group_T(pz[z1][:, 0:CAP], bi, j * 128, xgT, 0, CAP), r=xgk + (("wb", bi),), w=(("pz", z1),))
                            z3 = rot("b", pss)
                            S.add("pe", mm_group_T(pss[z3][:, 0:CAP], bi, 256 + j * 128, xgT, 0, CAP), r=xgk + (("wb", bi),), w=(("pss", z3),))
                            S.add("act", lambda e, z1=z1: e.activation(out=s1[:], in_=pz[z1][:, 0:CAP], func=AF.Silu),
                                  r=(("pz", z1),), w=("s1",))
                            S.add("dve", lambda e, z3=z3, fb=fb: e.tensor_tensor(out=hdT[:, fb, :], in0=s1[:], in1=pss[z3][:, 0:CAP], op=ALU.mult),
                                  r=("s1", ("pss", z3)), w=("hdT",))
                    for cu in range(D // 512):
                        bi = w13[0] % 2
                        w13[0] += 1
                        dst = wb[bi][:, 0:FKC, 0:512]
                        src = W2[o2:o2 + FE, cu * 512:(cu + 1) * 512].rearrange("(c p) n -> p c n", p=128)
                        dma(dst, src, r=("wfull",), w=(("wb", bi),), q="pool")
                        for tt in range(CT):
                            zi = rot("z", pz)
                            S.add("pe", mm_group(pz[zi][:, :], hdT, tt * 128, bi, 512, kc_n=FKC), r=("hdT", ("wb", bi)), w=(("pz", zi),))
                            yi = rot("s", yo)
                            S.add("act", lambda e, zi=zi, yi=yi: e.activation(out=yo[yi][:], in_=pz[zi][:, :], func=AF.Copy),
                                  r=(("pz", zi),), w=(("yo", yi),))
                            r0 = ex * CAP + tt * 128
                            dma(yg_d[r0:r0 + 128, cu * 512:(cu + 1) * 512], yo[yi][:], r=(("yo", yi),), w=("yg_d",))
                S.flush()
                if KSTOP == 8:
                    return nc

            with contextlib.ExitStack() as ef:
                def SF(name, shape, dt=F32):
                    return ef.enter_context(nc.sbuf_tensor(name, list(shape), dt))
                nfb = SF("nfb", [128, D])
                xnf = SF("xnf2", [128, D])
                ya1 = SF("ya1", [128, D])
                ya2 = SF("ya2", [128, D])
                dma(nfb[:], nfin_b.ap(), w=("nfb",))
                for t in range(NT):
                    r0 = t * 128
                    dma(xin[:], h2_d[r0:r0 + 128, :], w=("xin",), twice=True)
                    for k, dst in (((0, ya1), (1, ya2), (0, ya1), (1, ya2)) if KDUP else ((0, ya1), (1, ya2))):
                        S.add("pool", lambda e, k=k, t=t, dst=dst: e.indirect_dma_start(
                            out=dst[:], out_offset=None, in_=yg_d[:, :],
                            in_offset=bass.IndirectOffsetOnAxis(ap=idxs[:, t, k:k + 1], axis=0),
                            bounds_check=bound_reg(e), oob_is_err=False), r=("yg_d",), w=(("ya", k),), kind="d")
                        if k == 1 and KFENCE:
                            dma(fsb[:, 32:64], fence_d[:, 32:64], w=(("ya", 0), ("ya", 1), "fsb"), q="pool")
                    S.add("dve", lambda e, t=t: e.scalar_tensor_tensor(out=xin[:], in0=ya1[:], scalar=gates[:, t, 0:1], in1=xin[:],
                                                                       op0=ALU.mult, op1=ALU.add), r=(("ya", 0), "xin"), w=("xin",))
                    S.add("dve", lambda e, t=t: e.scalar_tensor_tensor(out=xin[:], in0=ya2[:], scalar=gates[:, t, 1:2], in1=xin[:],
                                                                       op0=ALU.mult, op1=ALU.add), r=(("ya", 1), "xin"), w=("xin",))
                    rstd_ops(xin[:], D, st[:, 0:1], st[:, 1:2], "xin", xsb[:], "xsb")
                    S.add("dve", lambda e: e.scalar_tensor_tensor(out=xnf[:], in0=xin[:], scalar=st[:, 1:2], in1=nfb[:],
                                                                  op0=ALU.mult, op1=ALU.mult), r=("xin", ("st", "rs"), "nfb"), w=("xnf",))
                    dma(out[r0:r0 + 128, :], xnf[:], r=("xnf",), w=("out",))
                S.flush()
                if KSTOP == 9:
                    return nc
    return nc


def host_inputs(cfg, inp):
    D, KC, TOK, DH, GH = cfg["D"], cfg["KC"], cfg["TOK"], cfg["DH"], cfg["GH"]
    SEQ, NE, FE = cfg["SEQ"], cfg["NE"], cfg["FE"]
    f = lambda a: np.ascontiguousarray(np.asarray(a, dtype=np.float32))
    x = f(inp["x"]).reshape(-1, D)
    memf = f(inp["mem"])
    ctab, _ = const_table(cfg)

    def cols(v):
        return f(v).reshape(KC, 128).T

    gcols = np.concatenate([cols(inp["n_mix"][0]), cols(inp["n_cross"][0]), cols(inp["n_mem"][0]), cols(inp["n_mem"][0])], axis=1)
    rep = lambda v: np.ascontiguousarray(np.broadcast_to(f(v).reshape(1, -1), (128, f(v).size)))
    lnp = np.concatenate([rep(inp["gmlp_ln_g"][0]), rep(inp["gmlp_ln_b"][0]), rep(inp["hgrn_lower_bounds"][0]),
                          rep(inp["hgrn_lower_bounds"][1]), rep(inp["hgrn_norm_g"][0])], axis=1)
    ws = f(inp["gmlp_w_s"][0])
    wsT = np.ascontiguousarray(ws.transpose(2, 0, 1).reshape(128, GH * 128))
    bsc = np.ascontiguousarray(f(inp["gmlp_b_s"][0]).T)
    wr = np.concatenate([f(inp["w_group"][0])] + [f(inp["w_router"][0][g]) for g in range(4)], axis=1)
    br = rep(np.concatenate([f(inp["b_group"][0]).reshape(-1), f(inp["b_router"][0]).reshape(-1)]))
    shared = dict(gcols=f(gcols), nmoe_b=rep(inp["n_moe"][0]), nfin_b=rep(inp["n_final"]), lnp=f(lnp), wsT=wsT, bsc=bsc,
                  wr=f(wr), br=br, consts=f(ctab))
    wfull = dict(w_in=f(inp["w_in"][0]), w_out=f(inp["w_out"][0]), w_q=f(inp["w_q_x"][0]), w_k=f(inp["w_k_x"][0]),
                 w_v=f(inp["w_v_x"][0]), w_o=f(inp["w_o_x"][0]),
                 w1=f(inp["w1_e"][0]).reshape(NE * D, FE), w3=f(inp["w3_e"][0]).reshape(NE * D, FE),
                 w2=f(inp["w2_e"][0]).reshape(NE * FE, D))
    maps = []
    per_seq = SEQ // TOK
    for c in range(NCORES):
        m = dict(shared)
        m["x_own"] = x[c * TOK:(c + 1) * TOK]
        if c % per_seq == 0:
            m["x_pre"] = np.zeros((TOK, D), np.float32)
        else:
            m["x_pre"] = x[(c - 1) * TOK:c * TOK]
        m["mem"] = memf[c // per_seq]
        for k, w in wfull.items():
            if not GATHER:
                m[k + "_s"] = w
            elif k in ("w1", "w3", "w2"):
                per = w.shape[0] // NE
                m[k + "_s"] = np.ascontiguousarray(np.concatenate([w[(8 * g + c) * per:(8 * g + c + 1) * per] for g in range(4)], axis=0))
            else:
                rows = w.shape[0] // NCORES
                m[k + "_s"] = w[c * rows:(c + 1) * rows]
        maps.append(m)
    return maps


_CACHE = {}


def run(cfg, inp):
    key = tuple(sorted(cfg.items()))
    if key not in _CACHE:
        _CACHE[key] = build(cfg)
    nc = _CACHE[key]
    maps = host_inputs(cfg, inp)
    res = run_bass_kernel_spmd(nc, maps, core_ids=list(range(NCORES)))
    if cfg.get("DEBUG"):
        return res.results
    outs = [res.results[c]["out"] for c in range(NCORES)]
    return np.concatenate(outs, axis=0).reshape(cfg["BATCH"], cfg["SEQ"], cfg["D"]).astype(np.float32)


def kernel(**inputs):
    cfg = make_cfg()
    return run(cfg, inputs)
```
